# Optimizing a Trainium2 kernel written in Bass

```python
import math
import jax, jax.numpy as jnp
from jax import lax
import numpy as np

D_MODEL = 1024
BATCH = 2
SEQ = 8192
DEPTH = 4

HEAD_DIM = 64
N_HEADS_A = 4
N_HEADS_B = 4
N_HEADS_C = 4
N_HEADS_D = 4
N_KV_D = 2
DIFF_DIM = HEAD_DIM // 2
IDX_HEADS = 8
IDX_DIM = 64
TOPK_MAX = 256
WINDOW = 128
BLOCK = 128
D_FF = 4 * D_MODEL
D_PLE = 256
ROPE_THETA = 10000.0
EPS = 1e-6
NEG_INF = -1e30
MIX_WIDTH = (N_HEADS_A + N_HEADS_B + N_HEADS_C + N_HEADS_D) * HEAD_DIM
SPLIT_WIDTHS = (
    N_HEADS_A * HEAD_DIM, N_HEADS_A * HEAD_DIM, N_HEADS_A * HEAD_DIM,
    IDX_HEADS * IDX_DIM, IDX_DIM, IDX_HEADS,
    N_HEADS_B * HEAD_DIM, N_HEADS_B * HEAD_DIM, N_HEADS_B * HEAD_DIM,
    N_HEADS_B,
    N_HEADS_C * HEAD_DIM, N_HEADS_C * HEAD_DIM, N_HEADS_C * HEAD_DIM,
    N_HEADS_D * HEAD_DIM, N_KV_D * HEAD_DIM, N_KV_D * HEAD_DIM,
)
D_IN = sum(SPLIT_WIDTHS)

kernel_name = 'hybrid_parallel_heads_dsa_fox_diff_swa'


def rmsnorm(x, g):
    xf = x.astype(jnp.float32)
    y = xf * lax.rsqrt(jnp.mean(xf * xf, axis=-1, keepdims=True) + EPS)
    return (y * g.astype(jnp.float32)).astype(x.dtype)


def rope_tables(positions, dim):
    half = dim // 2
    inv_freq = ROPE_THETA ** (-jnp.arange(half, dtype=jnp.float32) / half)
    ang = positions.astype(jnp.float32)[..., None] * inv_freq
    return jnp.cos(ang)[:, :, None, :], jnp.sin(ang)[:, :, None, :]


def apply_rope(x, tables):
    cos, sin = tables
    half = x.shape[-1] // 2
    xf = x.astype(jnp.float32)
    x1, x2 = xf[..., :half], xf[..., half:]
    return jnp.concatenate([x1 * cos - x2 * sin, x2 * cos + x1 * sin], axis=-1).astype(x.dtype)


def to_blocks(a):
    b, s = a.shape[:2]
    return jnp.moveaxis(a.reshape(b, s // BLOCK, BLOCK, *a.shape[2:]), 1, 0)


def from_blocks(o):
    nb, b = o.shape[:2]
    return jnp.moveaxis(o, 0, 1).reshape(b, nb * BLOCK, *o.shape[3:])


def dsa_attention(q, k, v, iq, ik, iw):
    s_len = k.shape[1]
    topk = min(TOPK_MAX, s_len // 4)
    key_pos = jnp.arange(s_len)
    scale = HEAD_DIM ** -0.5

    def one_block(args):
        qb, iqb, iwb, n = args
        t = n * BLOCK + jnp.arange(BLOCK)
        causal = key_pos[None, :] <= t[:, None]
        dots = jnp.einsum('bthd,bsd->bths', iqb, ik)
        score = jnp.einsum('bth,bths->bts', iwb, jax.nn.relu(dots)).astype(jnp.float32)
        score = jnp.where(causal[None], score, NEG_INF)
        _, idx = lax.top_k(score, topk)
        valid = idx <= t[None, :, None]
        k_sel = jax.vmap(lambda kk, ii: kk[ii])(k, idx)
        v_sel = jax.vmap(lambda vv, ii: vv[ii])(v, idx)
        logits = jnp.einsum('bthd,btjhd->bhtj', qb, k_sel).astype(jnp.float32) * scale
        logits = jnp.where(valid[:, None], logits, NEG_INF)
        probs = jax.nn.softmax(logits, axis=-1).astype(v.dtype)
        return jnp.einsum('bhtj,btjhd->bthd', probs, v_sel)

    nb = s_len // BLOCK
    out = lax.map(one_block, (to_blocks(q), to_blocks(iq), to_blocks(iw), jnp.arange(nb)))
    return from_blocks(out)


def forgetting_attention(q, k, v, log_f):
    s_len = k.shape[1]
    cum_bsh = jnp.cumsum(log_f, axis=1)
    cum = jnp.moveaxis(cum_bsh, 2, 1)
    key_pos = jnp.arange(s_len)
    scale = HEAD_DIM ** -0.5

    def one_block(args):
        qb, cb, n = args
        t = n * BLOCK + jnp.arange(BLOCK)
        causal = key_pos[None, :] <= t[:, None]
        logits = jnp.einsum('bthd,bshd->bhts', qb, k).astype(jnp.float32) * scale
        decay = jnp.moveaxis(cb, 2, 1)[..., :, None] - cum[:, :, None, :]
        logits = jnp.where(causal[None, None], logits + decay, NEG_INF)
        probs = jax.nn.softmax(logits, axis=-1).astype(v.dtype)
        return jnp.einsum('bhts,bshd->bthd', probs, v)

    nb = s_len // BLOCK
    out = lax.map(one_block, (to_blocks(q), to_blocks(cum_bsh), jnp.arange(nb)))
    return from_blocks(out)


def differential_attention(q, k, v, lam, lam_init, subln_g):
    s_len = k.shape[1]
    key_pos = jnp.arange(s_len)
    scale = DIFF_DIM ** -0.5

    def one_block(args):
        qb, n = args
        t = n * BLOCK + jnp.arange(BLOCK)
        causal = key_pos[None, :] <= t[:, None]
        logits = jnp.einsum('bthmd,bshmd->bmhts', qb, k).astype(jnp.float32) * scale
        logits = jnp.where(causal[None, None, None], logits, NEG_INF)
        probs = jax.nn.softmax(logits, axis=-1)
        diff = probs[:, 0] - lam * probs[:, 1]
        return jnp.einsum('bhts,bshd->bthd', diff.astype(v.dtype), v)

    nb = s_len // BLOCK
    out = from_blocks(lax.map(one_block, (to_blocks(q), jnp.arange(nb))))
    return rmsnorm(out, subln_g) * (1.0 - lam_init)


def sliding_window_sink_attention(q, k, v, sinks):
    b, s_len, n_h, dh = q.shape
    n_kv = k.shape[2]
    grp = n_h // n_kv
    nb = s_len // BLOCK
    qb = q.reshape(b, nb, BLOCK, n_kv, grp, dh)
    kb = k.reshape(b, nb, BLOCK, n_kv, dh)
    vb = v.reshape(b, nb, BLOCK, n_kv, dh)
    pad = ((0, 0), (1, 0), (0, 0), (0, 0), (0, 0))
    k_win = jnp.concatenate([jnp.pad(kb, pad)[:, :-1], kb], axis=2)
    v_win = jnp.concatenate([jnp.pad(vb, pad)[:, :-1], vb], axis=2)
    logits = jnp.einsum('bnikgd,bnjkd->bnkgij', qb, k_win).astype(jnp.float32) * (dh ** -0.5)
    i = jnp.arange(BLOCK)[:, None]
    j = jnp.arange(2 * BLOCK)[None, :]
    rel = i + BLOCK - j
    band = (rel >= 0) & (rel < WINDOW)
    in_range = (jnp.arange(nb)[:, None, None] > 0) | (j[None] >= BLOCK)
    mask = band[None] & in_range
    logits = jnp.where(mask[None, :, None, None], logits, NEG_INF)
    sink = jnp.broadcast_to(sinks.astype(jnp.float32).reshape(n_kv, grp)[None, None, :, :, None, None],
                            logits.shape[:-1] + (1,))
    probs = jax.nn.softmax(jnp.concatenate([logits, sink], axis=-1), axis=-1)[..., :-1]
    out = jnp.einsum('bnkgij,bnjkd->bnikgd', probs.astype(v.dtype), v_win)
    return out.reshape(b, s_len, n_h, dh)


def hybrid_layer(x, p_i, rope64, rope32, lam_init, w_in, b_forget, lq1, lk1, lq2, lk2,
                 diff_subln, sinks, w_out, g_pre_mix, g_post_mix, g_pre_mlp, g_post_mlp,
                 w_up, w_down, w_ple_proj, w_ple_gate):
    b, s_len, _ = x.shape
    h = rmsnorm(x, g_pre_mix)
    proj = h @ w_in
    points = np.cumsum(SPLIT_WIDTHS)[:-1].tolist()
    (a_q, a_k, a_v, a_iq, a_ik, a_iw, b_q, b_k, b_v, b_f,
     c_q, c_k, c_v, d_q, d_k, d_v) = jnp.split(proj, points, axis=-1)

    def heads(t, n, d):
        return t.reshape(b, s_len, n, d)

    iw = a_iw * (IDX_HEADS ** -0.5 * IDX_DIM ** -0.5)
    o_a = dsa_attention(apply_rope(heads(a_q, N_HEADS_A, HEAD_DIM), rope64),
                        apply_rope(heads(a_k, N_HEADS_A, HEAD_DIM), rope64),
                        heads(a_v, N_HEADS_A, HEAD_DIM),
                        apply_rope(heads(a_iq, IDX_HEADS, IDX_DIM), rope64),
                        apply_rope(a_ik[:, :, None, :], rope64)[:, :, 0],
                        iw)
    log_f = jax.nn.log_sigmoid((b_f + b_forget).astype(jnp.float32))
    o_b = forgetting_attention(heads(b_q, N_HEADS_B, HEAD_DIM), heads(b_k, N_HEADS_B, HEAD_DIM),
                               heads(b_v, N_HEADS_B, HEAD_DIM), log_f)
    cq = apply_rope(heads(c_q, 2 * N_HEADS_C, DIFF_DIM), rope32).reshape(b, s_len, N_HEADS_C, 2, DIFF_DIM)
    ck = apply_rope(heads(c_k, 2 * N_HEADS_C, DIFF_DIM), rope32).reshape(b, s_len, N_HEADS_C, 2, DIFF_DIM)
    f32 = jnp.float32
    lam = (jnp.exp(jnp.sum(lq1.astype(f32) * lk1.astype(f32)))
           - jnp.exp(jnp.sum(lq2.astype(f32) * lk2.astype(f32))) + lam_init)
    o_c = differential_attention(cq, ck, heads(c_v, N_HEADS_C, HEAD_DIM), lam, lam_init, diff_subln)
    o_d = sliding_window_sink_attention(apply_rope(heads(d_q, N_HEADS_D, HEAD_DIM), rope64),
                                        apply_rope(heads(d_k, N_KV_D, HEAD_DIM), rope64),
                                        heads(d_v, N_KV_D, HEAD_DIM), sinks)

    mix = jnp.concatenate([o_a.reshape(b, s_len, -1), o_b.reshape(b, s_len, -1),
                           o_c.reshape(b, s_len, -1), o_d.reshape(b, s_len, -1)], axis=-1)
    x = x + rmsnorm(mix @ w_out, g_post_mix)

    h = rmsnorm(x, g_pre_mlp)
    m = jnp.square(jax.nn.relu(h @ w_up)) @ w_down
    x = x + rmsnorm(m, g_post_mlp)

    x = x + jax.nn.sigmoid(x @ w_ple_gate) * (p_i @ w_ple_proj)
    return x


def setup_inputs(seed: int = 0) -> dict:
    key = jax.random.key(seed)
    ks = jax.random.split(key, 22)

    def normal(k, shape, scale):
        return jax.random.normal(k, shape, jnp.float32) * scale

    def gain(k, shape):
        return 1.0 + 0.02 * jax.random.normal(k, shape, jnp.float32)

    return {
        'x': normal(ks[0], (BATCH, SEQ, D_MODEL), 1.0),
        'p': normal(ks[1], (DEPTH, BATCH, SEQ, D_PLE), 1.0),
        'positions': jnp.broadcast_to(jnp.arange(SEQ, dtype=jnp.int32), (BATCH, SEQ)),
        'w_in': normal(ks[2], (DEPTH, D_MODEL, D_IN), D_MODEL ** -0.5),
        'b_forget': 2.0 + normal(ks[3], (DEPTH, N_HEADS_B), 0.5),
        'lambda_q1': normal(ks[4], (DEPTH, DIFF_DIM), 0.1),
        'lambda_k1': normal(ks[5], (DEPTH, DIFF_DIM), 0.1),
        'lambda_q2': normal(ks[6], (DEPTH, DIFF_DIM), 0.1),
        'lambda_k2': normal(ks[7], (DEPTH, DIFF_DIM), 0.1),
        'diff_subln': gain(ks[8], (DEPTH, HEAD_DIM)),
        'sinks': normal(ks[9], (DEPTH, N_HEADS_D), 1.0),
        'w_out': normal(ks[10], (DEPTH, MIX_WIDTH, D_MODEL), MIX_WIDTH ** -0.5),
        'norm_pre_mix': gain(ks[11], (DEPTH, D_MODEL)),
        'norm_post_mix': gain(ks[12], (DEPTH, D_MODEL)),
        'norm_pre_mlp': gain(ks[13], (DEPTH, D_MODEL)),
        'norm_post_mlp': gain(ks[14], (DEPTH, D_MODEL)),
        'w_mlp_up': normal(ks[15], (DEPTH, D_MODEL, D_FF), D_MODEL ** -0.5),
        'w_mlp_down': normal(ks[16], (DEPTH, D_FF, D_MODEL), D_FF ** -0.5),
        'w_ple_proj': normal(ks[17], (DEPTH, D_PLE, D_MODEL), D_PLE ** -0.5),
        'w_ple_gate': normal(ks[18], (DEPTH, D_MODEL, D_MODEL), D_MODEL ** -0.5),
    }


def reference(x, p, positions, w_in, b_forget, lambda_q1, lambda_k1, lambda_q2, lambda_k2,
              diff_subln, sinks, w_out, norm_pre_mix, norm_post_mix, norm_pre_mlp, norm_post_mlp,
              w_mlp_up, w_mlp_down, w_ple_proj, w_ple_gate):
    rope64 = rope_tables(positions, HEAD_DIM)
    rope32 = rope_tables(positions, DIFF_DIM)
    for i in range(DEPTH):
        lam_init = 0.8 - 0.6 * math.exp(-0.3 * i)
        x = hybrid_layer(x, p[i], rope64, rope32, lam_init, w_in[i], b_forget[i],
                         lambda_q1[i], lambda_k1[i], lambda_q2[i], lambda_k2[i],
                         diff_subln[i], sinks[i], w_out[i], norm_pre_mix[i], norm_post_mix[i],
                         norm_pre_mlp[i], norm_post_mlp[i], w_mlp_up[i], w_mlp_down[i],
                         w_ple_proj[i], w_ple_gate[i])
    return x
```

```python
import concourse.bass as bass
import concourse.mybir as mybir

COMPUTE = ("pe", "act", "dve", "pool")


class Buf:
    __slots__ = ("name", "writer", "readers")

    def __init__(self, name):
        self.name = name
        self.writer = None
        self.readers = []


class Op:
    __slots__ = ("eng", "fn", "deps", "signal", "token", "is_dma", "dsem", "idx")

    def __init__(self, eng, fn, is_dma, dsem):
        self.eng = eng
        self.fn = fn
        self.deps = []
        self.signal = False
        self.token = None
        self.is_dma = is_dma
        self.dsem = dsem


class Sched:
    def __init__(self, nc):
        self.nc = nc
        self.ops = {e: [] for e in ("pe", "act", "dve", "pool", "sp")}
        self.bufs = {}
        self.dsems = {}

    def buf(self, name):
        b = self.bufs.get(name)
        if b is None:
            b = self.bufs[name] = Buf(name)
        return b

    def _B(self, x):
        return x if isinstance(x, Buf) else self.buf(x)

    def op(self, eng, fn, reads=(), writes=(), dsem=None):
        is_dma = dsem is not None
        o = Op(eng, fn, is_dma, dsem)
        deps = []
        for r in reads:
            r = self._B(r)
            w = r.writer
            if w is not None:
                deps.append(w)
        for wb in writes:
            wb = self._B(wb)
            if wb.writer is not None:
                deps.append(wb.writer)
            deps.extend(wb.readers)
        seen = set()
        for d in deps:
            if id(d) in seen or d is o:
                continue
            seen.add(id(d))
            if (not is_dma) and (not d.is_dma) and d.eng == eng:
                if eng == "pe":
                    continue
                israw = any(self._B(r).writer is d for r in reads)
                if not israw:
                    continue
            d.signal = True
            o.deps.append(d)
        for r in reads:
            r = self._B(r)
            if not is_dma:
                r.readers = [x for x in r.readers if x.is_dma or x.eng != eng]
            r.readers.append(o)
        for wb in writes:
            wb = self._B(wb)
            wb.writer = o
            wb.readers = []
        self.ops[eng].append(o)
        return o

    def emit(self, final_waits=()):
        nc = self.nc
        import contextlib
        es = contextlib.ExitStack()
        with es:
            sems = {}
            for e in COMPUTE:
                sems[e] = es.enter_context(nc.semaphore("s_" + e))
            dnames = sorted({o.dsem for l in self.ops.values() for o in l if o.is_dma})
            for d in dnames:
                sems["d_" + d] = es.enter_context(nc.semaphore("d_" + d))
            cnt = {k: 0 for k in sems}
            for e, l in self.ops.items():
                for o in l:
                    if o.is_dma:
                        k = "d_" + o.dsem
                        o.signal = True
                    else:
                        k = e
            for e, l in self.ops.items():
                for o in l:
                    if not o.signal:
                        continue
                    k = ("d_" + o.dsem) if o.is_dma else e
                    cnt[k] += 16 if o.is_dma else 1
                    o.token = (k, cnt[k])
            block = es.enter_context(nc.Block())
            engobj = {"pe": block.tensor, "act": block.scalar, "dve": block.vector,
                      "pool": block.gpsimd, "sp": block.sync}
            final_waits = list(final_waits)

            def make(e):
                l = self.ops[e]

                def body(eng):
                    known = {}
                    for o in l:
                        for d in o.deps:
                            k, v = d.token
                            if known.get(k, 0) >= v:
                                continue
                            known[k] = v
                            eng.wait_ge(sems[k], v)
                        ins = o.fn(eng)
                        if o.signal:
                            k, v = o.token
                            ins.then_inc(sems[k], 16 if o.is_dma else 1)
                            if k == e:
                                pass
                    if e == "sp":
                        for o in final_waits:
                            k, v = o.token
                            if known.get(k, 0) < v:
                                known[k] = v
                                eng.wait_ge(sems[k], v)
                return body

            for e in ("sp", "act", "dve", "pool", "pe"):
                if self.ops[e] or e == "sp":
                    engobj[e](make(e))

import math, contextlib
import numpy as np
import concourse.bass as bass
import concourse.mybir as mybir

F32 = mybir.dt.float32
BF16 = mybir.dt.bfloat16
I32 = mybir.dt.int32
AF = mybir.ActivationFunctionType
ALU = mybir.AluOpType

D = 1024
NB = 16
NTOK = NB * 128
D_IN = 3404
EPS = 1e-6
TWO_PI = 2.0 * math.pi
C1 = 6.28125
C2 = TWO_PI - C1

_off = np.cumsum([0, 256, 256, 256, 512, 64, 8, 256, 256, 256, 4, 256, 256, 256, 256, 128, 128])
(A_Q, A_K, A_V, A_IQ, A_IK, A_IW, B_Q, B_K, B_V, B_F, C_Q, C_K, C_V, D_Q, D_K, D_V) = [int(v) for v in _off[:16]]


def build_perm():
    p = []
    r = lambda s, n: list(range(s, s + n))
    p += r(A_Q, 256) + r(A_K, 256) + r(A_IQ, 512) + r(A_IK, 64) + r(A_IK, 64) + r(D_Q, 256)
    p += r(D_K, 64) + r(D_K, 64) + r(D_K + 64, 64) + r(D_K + 64, 64)
    p += r(C_Q, 256) + r(C_K, 256)
    for h in range(4):
        p += r(B_Q + 64 * h, 64) + [-1] * 64
    for h in range(4):
        p += r(B_K + 64 * h, 64) + [-1] * 64
    p += r(A_V, 256) + r(B_V, 256) + r(C_V, 256) + r(D_V, 128)
    p += r(A_IW, 8) + r(B_F, 4)
    return np.array(p, dtype=np.int64)


PERM = build_perm()
WP = len(PERM)
NFT = 25
R64 = 1664
R32 = 2176
NT = 3200
VOFF = 3200
SOFF = 4096
IW_SCALE = (8 ** -0.5) * (64 ** -0.5)


def permute_w_in(w_in_l):
    w = np.zeros((D, WP), dtype=np.float32)
    m = PERM >= 0
    w[:, m] = w_in_l[:, PERM[m]]
    return w


def build_p():
    nc = bass.Bass("TRN2", target_bir_lowering=False)
    x = nc.dram_tensor("x", [NTOK, D], F32, kind="ExternalInput").ap()
    w = nc.dram_tensor("w", [D, WP], F32, kind="ExternalInput").ap()
    g = nc.dram_tensor("g", [128, 8], F32, kind="ExternalInput").ap()
    bfg = nc.dram_tensor("bfg", [1, 4], F32, kind="ExternalInput").ap()
    pos = nc.dram_tensor("pos", [128, NB], I32, kind="ExternalInput").ap()
    invf = nc.dram_tensor("invf", [1, 48], F32, kind="ExternalInput").ap()
    ft = nc.dram_tensor("ft", [NFT, 128, NTOK], BF16, kind="ExternalOutput").ap()
    vv = nc.dram_tensor("vv", [NTOK, 2048], BF16, kind="ExternalOutput").ap()
    sm = nc.dram_tensor("sm", [NTOK, 12], F32, kind="ExternalOutput").ap()
    es = contextlib.ExitStack()
    with es:
        def sb(name, shape, dt=F32):
            return es.enter_context(nc.sbuf_tensor(name, shape, dt))

        def ps(name, shape, dt=F32):
            return es.enter_context(nc.psum_tensor(name, shape, dt))

        S = Sched(nc)
        wbf = sb("wbf", [128, 8, WP], BF16)
        wst = [sb(f"wst{i}", [128, 1024], F32) for i in range(2)]
        gt = sb("gt", [128, 8])
        bft = sb("bft", [128, 4])
        post = sb("post", [128, NB], I32)
        posf = sb("posf", [128, NB])
        invt = sb("invt", [128, 48])
        identf = sb("identf", [128, 128])
        ident = sb("ident", [128, 128], BF16)
        ang = sb("ang", [128, NB, 48])
        ang2 = sb("ang2", [128, NB, 48])
        ki = sb("ki", [128, NB, 48], I32)
        kf = sb("kf", [128, NB, 48])
        sint = sb("sint", [128, NB, 48])
        cost = sb("cost", [128, NB, 48])
        xt = [sb(f"xt{i}", [128, D]) for i in range(2)]
        junk = sb("junk", [128, D], BF16)
        ss = [sb(f"ss{i}", [128, 1]) for i in range(2)]
        sq = [sb(f"sq{i}", [128, 1]) for i in range(2)]
        rstd = [sb(f"rstd{i}", [128, 1]) for i in range(2)]
        hb = [sb(f"hb{i}", [128, D], BF16) for i in range(2)]
        hT = [sb(f"hT{i}", [128, D], BF16) for i in range(2)]
        proj = [sb(f"proj{i}", [128, WP]) for i in range(2)]
        rot = [sb(f"rot{i}", [128, NT], BF16) for i in range(2)]
        t1 = sb("t1", [128, 832]); t2 = sb("t2", [128, 832])
        t3 = sb("t3", [128, 832]); t4 = sb("t4", [128, 832])
        ftsb = [sb(f"ftsb{i}", [128, NFT, 128], BF16) for i in range(2)]
        vsb = [sb(f"vsb{i}", [128, 2048], BF16) for i in range(2)]
        smt = [sb(f"smt{i}", [128, 12]) for i in range(2)]
        za = sb("za", [128, 4]); zb = sb("zb", [128, 4]); zc = sb("zc", [128, 4]); zd = sb("zd", [128, 4])
        epst = sb("epst", [128, 1])
        pT = [ps(f"pT{i}", [128, D], BF16) for i in range(2)]
        pp = [ps(f"pp{i}", [128, 512]) for i in range(2)]
        pF = [ps(f"pF{i}", [128, 1024], BF16) for i in range(4)]

        S.op("sp", lambda e: e.dma_start(out=gt[:], in_=g[:, :]), writes=["gt"], dsem="c0")
        S.op("sp", lambda e: e.dma_start(out=bft[:], in_=bfg[0:1, :].to_broadcast([128, 4])), writes=["bft"], dsem="c1")
        S.op("sp", lambda e: e.dma_start(out=post[:], in_=pos[:, :]), writes=["post"], dsem="c2")
        S.op("sp", lambda e: e.dma_start(out=invt[:], in_=invf[0:1, :].to_broadcast([128, 48])), writes=["invt"], dsem="c3")
        S.op("pool", lambda e: e.memset(identf[:], 0.0), writes=["identf"])
        S.op("pool", lambda e: e.affine_select(out=identf[:], in_=identf[:], pattern=[[-1, 128]],
                                               compare_op=ALU.not_equal, fill=1.0, base=0, channel_multiplier=1),
             reads=["identf"], writes=["identf"])
        S.op("dve", lambda e: e.tensor_copy(out=ident[:], in_=identf[:]), reads=["identf"], writes=["ident"])
        S.op("pool", lambda e: e.memset(epst[:], EPS), writes=["epst"])
        onest = sb("onest", [128, 1])
        S.op("pool", lambda e: e.memset(onest[:], 1.0), writes=["onest"])
        NWC = (WP + 1023) // 1024
        i = 0
        for kc in range(8):
            for c in range(NWC):
                c0 = c * 1024
                cw = min(1024, WP - c0)
                st = wst[i % 2]
                sname = f"wst{i % 2}"
                S.op("sp", lambda e, st=st, kc=kc, c0=c0, cw=cw: e.dma_start(out=st[:, 0:cw], in_=w[kc * 128:(kc + 1) * 128, c0:c0 + cw]),
                     writes=[sname], dsem=sname)
                eng = "dve" if i % 2 == 0 else "pool"
                S.op(eng, lambda e, st=st, kc=kc, c0=c0, cw=cw: e.tensor_scalar(out=wbf[:, kc, c0:c0 + cw], in0=st[:, 0:cw], scalar1=gt[:, kc:kc + 1], scalar2=None, op0=ALU.mult),
                     reads=[sname, "gt"], writes=[f"wbf{kc}_{c}"])
                i += 1
        wbf_bufs = [f"wbf{kc}_{c}" for kc in range(8) for c in range(NWC)]
        S.op("dve", lambda e: e.tensor_copy(out=posf[:], in_=post[:]), reads=["post"], writes=["posf"])
        S.op("dve", lambda e: e.tensor_tensor(out=ang[:], in0=posf[:, :].unsqueeze(2).to_broadcast([128, NB, 48]),
                                              in1=invt[:, :].unsqueeze(1).to_broadcast([128, NB, 48]), op=ALU.mult),
             reads=["posf", "invt"], writes=["ang"])
        S.op("dve", lambda e: e.tensor_scalar(out=ang2[:], in0=ang[:], scalar1=math.pi / 2, scalar2=None, op0=ALU.add),
             reads=["ang"], writes=["ang2"])
        for (a, aname, outt, oname) in ((ang, "ang", sint, "sint"), (ang2, "ang2", cost, "cost")):
            S.op("dve", lambda e, a=a: e.tensor_scalar(out=ki[:], in0=a[:], scalar1=1.0 / TWO_PI, scalar2=None, op0=ALU.mult),
                 reads=[aname], writes=["ki"])
            S.op("dve", lambda e: e.tensor_copy(out=kf[:], in_=ki[:]), reads=["ki"], writes=["kf"])
            S.op("dve", lambda e, a=a: e.scalar_tensor_tensor(out=a[:], in0=kf[:], scalar=-C1, in1=a[:], op0=ALU.mult, op1=ALU.add),
                 reads=["kf", aname], writes=[aname])
            S.op("dve", lambda e, a=a: e.scalar_tensor_tensor(out=a[:], in0=kf[:], scalar=-C2, in1=a[:], op0=ALU.mult, op1=ALU.add),
                 reads=["kf", aname], writes=[aname])
            S.op("dve", lambda e, a=a: e.tensor_scalar(out=a[:], in0=a[:], scalar1=-3.1415925, scalar2=3.1415925, op0=ALU.max, op1=ALU.min),
                 reads=[aname], writes=[aname])
            S.op("act", lambda e, a=a, outt=outt: e.activation(out=outt[:], in_=a[:], func=AF.Sin), reads=[aname], writes=[oname])

        outs = []
        for m in range(NB):
            b = m % 2
            X, HB, HT, PJ, RT = xt[b], hb[b], hT[b], proj[b], rot[b]
            n = lambda s: f"{s}{b}"
            S.op("sp", lambda e, X=X, m=m: e.dma_start(out=X[:], in_=x[m * 128:(m + 1) * 128, :]), writes=[n("xt")], dsem=n("xt"))
            S.op("act", lambda e, X=X, b=b: e.activation(out=junk[:], in_=X[:], func=AF.Square, accum_out=ss[b][:]),
                 reads=[n("xt")], writes=["junk", n("ss")])
            S.op("act", lambda e, b=b: e.activation(out=sq[b][:], in_=ss[b][:], func=AF.Sqrt, scale=1.0 / D, bias=epst[:]),
                 reads=[n("ss"), "epst"], writes=[n("sq")])
            S.op("dve", lambda e, b=b: e.reciprocal(out=rstd[b][:], in_=sq[b][:]), reads=[n("sq")], writes=[n("rstd")])
            S.op("act", lambda e, X=X, HB=HB, b=b: e.activation(out=HB[:], in_=X[:], func=AF.Copy, scale=rstd[b][:]),
                 reads=[n("xt"), n("rstd")], writes=[n("hb")])
            for kc in range(8):
                S.op("pe", lambda e, HB=HB, b=b, kc=kc: e.transpose(pT[b][:, kc * 128:(kc + 1) * 128], HB[:, kc * 128:(kc + 1) * 128], ident[:]),
                     reads=[n("hb"), "ident"], writes=[n("pT")])
            S.op("dve", lambda e, HT=HT, b=b: e.tensor_copy(out=HT[:], in_=pT[b][:]), reads=[n("pT")], writes=[n("hT")])
            NCH = (WP + 511) // 512
            for c in range(NCH):
                c0 = c * 512
                cw = min(512, WP - c0)
                pb = c % 2
                for kc in range(8):
                    S.op("pe", lambda e, HT=HT, pb=pb, kc=kc, c0=c0, cw=cw: e.matmul(pp[pb][:, 0:cw], HT[:, kc * 128:(kc + 1) * 128], wbf[:, kc, c0:c0 + cw], start=(kc == 0), stop=(kc == 7)),
                         reads=[n("hT")] + (wbf_bufs if (m == 0) else []), writes=[f"pp{pb}"])
                eng = "act" if c % 2 == 0 else "dve"
                if eng == "act":
                    S.op("act", lambda e, PJ=PJ, pb=pb, c0=c0, cw=cw: e.activation(out=PJ[:, c0:c0 + cw], in_=pp[pb][:, 0:cw], func=AF.Copy),
                         reads=[f"pp{pb}"], writes=[n("proj") + f"_{c}"])
                else:
                    S.op("dve", lambda e, PJ=PJ, pb=pb, c0=c0, cw=cw: e.tensor_copy(out=PJ[:, c0:c0 + cw], in_=pp[pb][:, 0:cw]),
                         reads=[f"pp{pb}"], writes=[n("proj") + f"_{c}"])
            pj_all = [n("proj") + f"_{c}" for c in range(NCH)]
            def rope(lo, nh, half, toff):
                width = nh * half
                Xv = PJ[:, lo:lo + 2 * width].rearrange("p (h two d) -> p h two d", two=2, d=half)
                Ov = RT[:, lo:lo + 2 * width].rearrange("p (h two d) -> p h two d", two=2, d=half)
                x1, x2 = Xv[:, :, 0, :], Xv[:, :, 1, :]
                o1, o2 = Ov[:, :, 0, :], Ov[:, :, 1, :]
                cs = cost[:, m, toff:toff + half].unsqueeze(1).to_broadcast([128, nh, half])
                sn = sint[:, m, toff:toff + half].unsqueeze(1).to_broadcast([128, nh, half])
                v = lambda t: t[:, 0:width].rearrange("p (h d) -> p h d", d=half)
                S.op("dve", lambda e: e.tensor_tensor(out=v(t1), in0=x1, in1=cs, op=ALU.mult), reads=pj_all + ["cost"], writes=["t1"])
                S.op("pool", lambda e: e.tensor_tensor(out=v(t2), in0=x2, in1=sn, op=ALU.mult), reads=pj_all + ["sint"], writes=["t2"])
                S.op("dve", lambda e: e.tensor_tensor(out=o1, in0=v(t1), in1=v(t2), op=ALU.subtract), reads=["t1", "t2"], writes=[n("rot") + f"a{lo}"])
                S.op("pool", lambda e: e.tensor_tensor(out=v(t3), in0=x2, in1=cs, op=ALU.mult), reads=pj_all + ["cost"], writes=["t3"])
                S.op("dve", lambda e: e.tensor_tensor(out=v(t4), in0=x1, in1=sn, op=ALU.mult), reads=pj_all + ["sint"], writes=["t4"])
                S.op("pool", lambda e: e.tensor_tensor(out=o2, in0=v(t3), in1=v(t4), op=ALU.add), reads=["t3", "t4"], writes=[n("rot") + f"b{lo}"])
            rope(0, 26, 32, 0)
            rope(R64, 16, 16, 32)
            S.op("act", lambda e, PJ=PJ, RT=RT: e.activation(out=RT[:, R32:NT], in_=PJ[:, R32:NT], func=AF.Copy),
                 reads=pj_all, writes=[n("rot") + "c"])
            S.op("act", lambda e, RT=RT: e.activation(out=RT[:, 2688:3200].rearrange("p (h c) -> p h c", c=128)[:, :, 64:65], in_=RT[:, 2688:3200].rearrange("p (h c) -> p h c", c=128)[:, :, 64:65], func=AF.Identity, scale=0.0, bias=onest[:]),
                 reads=[n("rot") + "c", "onest"], writes=[n("rot") + "c"])
            rot_all = [n("rot") + f"a0", n("rot") + f"b0", n("rot") + f"a{R64}", n("rot") + f"b{R64}", n("rot") + "c"]
            for c in range(NFT):
                bank = c // 8
                S.op("pe", lambda e, RT=RT, c=c, bank=bank: e.transpose(pF[bank][:, (c % 8) * 128:(c % 8 + 1) * 128], RT[:, c * 128:(c + 1) * 128], ident[:]),
                     reads=rot_all + ["ident"], writes=[f"pF{bank}"])
            for bank in range(4):
                nt = min(8, NFT - bank * 8)
                eng = "act" if bank % 2 == 0 else "dve"
                dst = ftsb[b][:, bank * 8:bank * 8 + nt, :]
                src = pF[bank][:, 0:nt * 128].rearrange("p (c t) -> p c t", t=128)
                if eng == "act":
                    S.op("act", lambda e, dst=dst, src=src: e.activation(out=dst, in_=src, func=AF.Copy), reads=[f"pF{bank}"], writes=[n("ftsb") + f"_{bank}"])
                else:
                    S.op("dve", lambda e, dst=dst, src=src: e.tensor_copy(out=dst, in_=src), reads=[f"pF{bank}"], writes=[n("ftsb") + f"_{bank}"])
            o = S.op("sp", lambda e, b=b, m=m: e.dma_start(out=ft[:, :, m * 128:(m + 1) * 128].rearrange("c p t -> p c t"), in_=ftsb[b][:, :, :]),
                     reads=[n("ftsb") + f"_{k}" for k in range(4)], dsem=n("oft"))
            outs.append(o)
            if m < 2:
                S.op("pool", lambda e, b=b: e.memset(vsb[b][:], 1.0), writes=[n("vsb")])
            def vcopy(b=b, PJ=PJ):
                dv = vsb[b][:, 0:1536].rearrange("p (a two c) -> p a two c", two=2, c=128)
                sv = PJ[:, VOFF:VOFF + 768].rearrange("p (a two c) -> p a two c", two=2, c=64)
                S.op("pool", lambda e: e.tensor_copy(out=dv[:, :, 0, 0:64], in_=sv[:, :, 0, :]), reads=pj_all + [n("vsb")], writes=[n("vsb")])
                S.op("pool", lambda e: e.tensor_copy(out=dv[:, :, 1, 64:128], in_=sv[:, :, 1, :]), reads=pj_all + [n("vsb")], writes=[n("vsb")])
                dd = vsb[b][:, 1536:2048].rearrange("p (k two c) -> p k two c", two=2, c=128)
                sd = PJ[:, VOFF + 768:VOFF + 896].rearrange("p (k c) -> p k c", c=64)
                S.op("pool", lambda e: e.tensor_copy(out=dd[:, :, 0, 0:64], in_=sd), reads=pj_all + [n("vsb")], writes=[n("vsb")])
                S.op("pool", lambda e: e.tensor_copy(out=dd[:, :, 1, 64:128], in_=sd), reads=pj_all + [n("vsb")], writes=[n("vsb")])
            vcopy()
            o = S.op("sp", lambda e, b=b, m=m: e.dma_start(out=vv[m * 128:(m + 1) * 128, :], in_=vsb[b][:]), reads=[n("vsb")], dsem=n("ov"))
            outs.append(o)
            S.op("dve", lambda e, PJ=PJ, b=b: e.tensor_scalar(out=smt[b][:, 0:8], in0=PJ[:, SOFF:SOFF + 8], scalar1=IW_SCALE, scalar2=None, op0=ALU.mult),
                 reads=pj_all, writes=[n("smt") + "a"])
            S.op("dve", lambda e, PJ=PJ: e.tensor_tensor(out=za[:], in0=PJ[:, SOFF + 8:SOFF + 12], in1=bft[:], op=ALU.add), reads=pj_all + ["bft"], writes=["za"])
            S.op("act", lambda e: e.activation(out=zb[:], in_=za[:], func=AF.Abs), reads=["za"], writes=["zb"])
            S.op("act", lambda e: e.activation(out=zc[:], in_=zb[:], func=AF.Exp, scale=-1.0), reads=["zb"], writes=["zc"])
            S.op("act", lambda e: e.activation(out=zd[:], in_=zc[:], func=AF.Ln, bias=1.0), reads=["zc"], writes=["zd"])
            S.op("dve", lambda e: e.tensor_scalar(out=zb[:], in0=za[:], scalar1=0.0, scalar2=None, op0=ALU.min), reads=["za", "zc"], writes=["zb"])
            S.op("dve", lambda e, b=b: e.tensor_tensor(out=smt[b][:, 8:12], in0=zb[:], in1=zd[:], op=ALU.subtract), reads=["zb", "zd"], writes=[n("smt") + "b"])
            o = S.op("sp", lambda e, b=b, m=m: e.dma_start(out=sm[m * 128:(m + 1) * 128, :], in_=smt[b][:]), reads=[n("smt") + "a", n("smt") + "b"], dsem=n("osm"))
            outs.append(o)
        S.emit(final_waits=outs)
    return nc


def invf_table():
    f64 = 10000.0 ** (-np.arange(32, dtype=np.float32) / 32)
    f32 = 10000.0 ** (-np.arange(16, dtype=np.float32) / 16)
    return np.concatenate([f64, f32]).astype(np.float32)[None, :]

import math, contextlib
import numpy as np
import concourse.bass as bass
import concourse.mybir as mybir

F32 = mybir.dt.float32
BF16 = mybir.dt.bfloat16
AF = mybir.ActivationFunctionType
ALU = mybir.AluOpType
AX = mybir.AxisListType

NITER = 14
NEG = -30000.0
SC_A = 0.125
SC_C = 32 ** -0.5
QA, QI, QD, QC, QB = 0, 2, 6, 8, 10
KA, KI, KD, KC, KB = 0, 2, 3, 5, 7
VA, VB, VC, VD = 0, 4, 8, 12


def build_q1(groups=("A", "B", "C", "D"), gs=(0, 1, 2, 3)):
    nc = bass.Bass("TRN2", target_bir_lowering=False)
    I = lambda name, shape, dt=F32: nc.dram_tensor(name, shape, dt, kind="ExternalInput").ap()
    ftq = I("ftq", [14, 128, 2048], BF16)
    ftk = I("ftk", [4, 11, 128, 2048], BF16)
    vk = I("vk", [4, 2048, 2048], BF16)
    lfa = I("lfa", [128, 64, 4])
    iwo = I("iwo", [128, 16, 8])
    selb = I("selb", [64, 16])
    cmask = I("cmask", [128, 16, 512], BF16)
    idxm = I("idxm", [128, 2, 512])
    dmask = I("dmask", [128, 5, 128], BF16)
    lamr = I("lamr", [1, 128])
    cst = I("cst", [128, 9])
    mixT = nc.dram_tensor("mixT", [8, 128, 2048], BF16, kind="ExternalOutput").ap()
    es = contextlib.ExitStack()
    with es:
        def sb(name, shape, dt=F32):
            return es.enter_context(nc.sbuf_tensor(name, shape, dt))

        def ps(name, shape, dt=F32):
            return es.enter_context(nc.psum_tensor(name, shape, dt))

        S = Sched(nc)
        MIX = sb("MIX", [128, 8, 512], BF16)
        QS = sb("QS", [128, 14, 512], BF16)
        CQZ = sb("CQZ", [128, 2, 2, 512], BF16)
        kt = [sb(f"kt{i}", [128, 4, 4, 128], BF16) for i in range(2)]
        vt = [sb(f"vt{i}", [128, 4, 4, 128], BF16) for i in range(2)]
        score = sb("score", [128, 8192])
        mk = sb("mk", [128, 8192], BF16)
        mbt = sb("mbt", [128, 64, 128], BF16)
        R = [sb(f"R{i}", [128, 512], BF16) for i in range(4)]
        diag = sb("diag", [128, 8, 128], BF16)
        PT = [sb(f"PT{i}", [128, 512], BF16) for i in range(4)]
        PM = [sb(f"PM{i}", [128, 512], BF16) for i in range(2)]
        cm = sb("cm", [128, 16, 512], BF16)
        ixm = sb("ixm", [128, 2, 512])
        dm = sb("dm", [128, 5, 128], BF16)
        lfat = sb("lfat", [128, 64, 4])
        iwt = sb("iwt", [128, 16, 8])
        selt = sb("selt", [64, 16]); seltot = sb("seltot", [64, 16, 4])
        lamt = sb("lamt", [128, 128])
        cstt = sb("cstt", [128, 9]); subeff = sb("subeff", [128, 1])
        identf = sb("identf", [128, 128]); ident = sb("ident", [128, 128], BF16)
        trif = sb("trif", [128, 128]); onesf = sb("onesf", [128, 128])
        bones = sb("bones", [128, 128], BF16)
        onesb = sb("onesb", [128, 128], BF16)
        totT = sb("totT", [64, 4]); Bt = sb("Bt", [64, 64, 4])
        nb = sb("nb", [128, 64, 4]); crefsc = sb("crefsc", [128, 16, 4])
        lprod = sb("lprod", [128, 64]); lsum = sb("lsum", [128, 2]); lexp = sb("lexp", [128, 2])
        lam = sb("lam", [128, 1]); nlam = sb("nlam", [128, 1])
        esk = sb("esk", [128, 4]); skrow = sb("skrow", [1, 4, 128], BF16)
        pw = sb("pw", [128, NITER])
        hi = sb("hi", [128, 1]); lo = [sb(f"lo{i}", [128, 1]) for i in range(2)]
        mn1 = sb("mn1", [128, 1]); mn2 = sb("mn2", [128, 1]); w0 = sb("w0", [128, 1]); W = sb("W", [128, NITER])
        mid = sb("mid", [128, 1]); cnt = sb("cnt", [128, 1]); ge = sb("ge", [128, 1]); gw = sb("gw", [128, 1])
        lastm = sb("lastm", [128, 512])
        dl1 = sb("dl1", [128, 128], BF16); dl2 = sb("dl2", [128, 128], BF16)
        rs = sb("rs", [128, 512]); rs2 = sb("rs2", [128, 512])
        o1 = sb("o1", [128, 512]); o2 = sb("o2", [128, 512]); dsq = sb("dsq", [128, 512], BF16)
        rt = sb("rt", [128, 512]); epst = sb("epst", [128, 1])
        S0 = ps("S0", [128, 512]); S1 = ps("S1", [128, 512])
        X0 = ps("X0", [128, 512]); X1 = ps("X1", [128, 1024], BF16)
        AC = [ps(f"AC{i}", [128, 512]) for i in range(4)]

        D = lambda o, i_, rd=(), wr=(), sem=None: S.op("sp", lambda e: e.dma_start(out=o, in_=i_), reads=rd, writes=wr, dsem=sem)
        D(cm[:], cmask[:, :, :], wr=["cm"], sem="c_cm")
        D(ixm[:], idxm[:, :, :], wr=["ixm"], sem="c_ixm")
        D(dm[:], dmask[:, :, :], wr=["dm"], sem="c_dm")
        D(lfat[:], lfa[:, :, :], wr=["lfat"], sem="c_lfa")
        D(iwt[:], iwo[:, :, :], wr=["iwt"], sem="c_iw")
        D(selt[:], selb[:, :], wr=["selt"], sem="c_sel")
        D(lamt[:], lamr[0:1, :].to_broadcast([128, 128]), wr=["lamt"], sem="c_lam")
        D(cstt[:], cst[:, :], wr=["cstt"], sem="c_cst")
        P = lambda fn, rd=(), wr=(): S.op("pool", fn, reads=rd, writes=wr)
        V = lambda fn, rd=(), wr=(): S.op("dve", fn, reads=rd, writes=wr)
        A = lambda fn, rd=(), wr=(): S.op("act", fn, reads=rd, writes=wr)
        T = lambda fn, rd=(), wr=(): S.op("pe", fn, reads=rd, writes=wr)
        P(lambda e: e.memset(identf[:], 0.0), wr=["identf"])
        P(lambda e: e.affine_select(out=identf[:], in_=identf[:], pattern=[[-1, 128]], compare_op=ALU.not_equal, fill=1.0, base=0, channel_multiplier=1), rd=["identf"], wr=["identf"])
        V(lambda e: e.tensor_copy(out=ident[:], in_=identf[:]), rd=["identf"], wr=["ident"])
        P(lambda e: e.memset(onesf[:], 1.0), wr=["onesf"])
        P(lambda e: e.memset(onesb[:], 1.0), wr=["onesb"])
        P(lambda e: e.memset(epst[:], 1e-6), wr=["epst"])
        P(lambda e: e.memset(trif[:], 1.0), wr=["trif"])
        P(lambda e: e.affine_select(out=trif[:], in_=trif[:], pattern=[[1, 128]], compare_op=ALU.is_ge, fill=0.0, base=0, channel_multiplier=-1), rd=["trif"], wr=["trif"])
        P(lambda e: e.memset(bones[:], 0.0), wr=["bones"])
        P(lambda e: e.memset(bones[0:64, 0:64], 1.0), rd=["bones"], wr=["bones"])
        P(lambda e: e.memset(bones[64:128, 64:128], 1.0), rd=["bones"], wr=["bones"])
        for k in range(NITER):
            P(lambda e, k=k: e.memset(pw[:, k:k + 1], 2.0 ** -(k + 1)), wr=["pw"])
        for h in range(4):
            T(lambda e, h=h: e.matmul(X0[0:64, h:h + 1], lfat[:, :, h], onesf[:, 0:1], start=True, stop=True), rd=["lfat", "onesf"], wr=["X0"])
        V(lambda e: e.tensor_copy(out=totT[:], in_=X0[0:64, 0:4]), rd=["X0"], wr=["totT"])
        V(lambda e: e.tensor_copy(out=Bt[:], in_=totT[:, :].unsqueeze(1).to_broadcast([64, 64, 4])), rd=["totT"], wr=["Bt"])
        P(lambda e: e.affine_select(out=Bt[:], in_=Bt[:], pattern=[[1, 64], [0, 4]], compare_op=ALU.is_gt, fill=0.0, base=0, channel_multiplier=-1), rd=["Bt"], wr=["Bt"])
        T(lambda e: e.matmul(AC[0][:, 0:256], trif[:, :], lfat[:].rearrange("p b h -> p (b h)"), start=True, stop=False), rd=["trif", "lfat"], wr=["AC0"])
        T(lambda e: e.matmul(AC[0][:, 0:256], onesf[0:64, :], Bt[:].rearrange("p b h -> p (b h)"), start=False, stop=True), rd=["onesf", "Bt"], wr=["AC0"])
        A(lambda e: e.activation(out=nb[:].rearrange("p b h -> p (b h)"), in_=AC[0][:, 0:256], func=AF.Copy, scale=-1.0), rd=["AC0"], wr=["nb"])
        V(lambda e: e.tensor_tensor(out=seltot[:], in0=selt[:, :].unsqueeze(2).to_broadcast([64, 16, 4]), in1=totT[:, :].unsqueeze(1).to_broadcast([64, 16, 4]), op=ALU.mult), rd=["selt", "totT"], wr=["seltot"])
        T(lambda e: e.matmul(AC[1][:, 0:64], onesf[0:64, :], seltot[:].rearrange("p m h -> p (m h)"), start=True, stop=True), rd=["onesf", "seltot"], wr=["AC1"])
        A(lambda e: e.activation(out=crefsc[:].rearrange("p m h -> p (m h)"), in_=AC[1][:, 0:64], func=AF.Copy, scale=1.0 / SC_A), rd=["AC1"], wr=["crefsc"])
        V(lambda e: e.tensor_tensor(out=lprod[:], in0=lamt[:, 0:64], in1=lamt[:, 64:128], op=ALU.mult), rd=["lamt"], wr=["lprod"])
        V(lambda e: e.tensor_reduce(out=lsum[:], in_=lprod[:].rearrange("p (a b) -> p a b", b=32), axis=AX.X, op=ALU.add), rd=["lprod"], wr=["lsum"])
        A(lambda e: e.activation(out=lexp[:], in_=lsum[:], func=AF.Exp), rd=["lsum"], wr=["lexp"])
        V(lambda e: e.tensor_tensor(out=lam[:], in0=lexp[:, 0:1], in1=lexp[:, 1:2], op=ALU.subtract), rd=["lexp"], wr=["lam"])
        V(lambda e: e.tensor_tensor(out=lam[:], in0=lam[:], in1=cstt[:, 4:5], op=ALU.add), rd=["lam", "cstt"], wr=["lam"])
        V(lambda e: e.tensor_scalar(out=nlam[:], in0=lam[:], scalar1=-1.0, scalar2=None, op0=ALU.mult), rd=["lam"], wr=["nlam"])
        A(lambda e: e.activation(out=esk[:], in_=cstt[:, 0:4], func=AF.Exp), rd=["cstt"], wr=["esk"])
        V(lambda e: e.tensor_tensor(out=subeff[:], in0=cstt[:, 5:6], in1=cstt[:, 8:9], op=ALU.mult), rd=["cstt"], wr=["subeff"])
        P(lambda e: e.memset(skrow[:], 0.0), wr=["skrow"])
        for hq in range(4):
            c0 = 64 if hq % 2 == 0 else 0
            V(lambda e, hq=hq, c0=c0: e.tensor_scalar(out=skrow[0:1, hq, c0:c0 + 64], in0=onesf[0:1, 0:64], scalar1=esk[0:1, hq:hq + 1], scalar2=None, op0=ALU.mult),
              rd=["skrow", "esk", "onesf"], wr=["skrow"])

        ldc = [0]

        def load_k(tiles, kc, nr=4, roff=0, m_of_r=None):
            b = ldc[0] % 2
            ldc[0] += 1
            for ti, tid in enumerate(tiles):
                D(kt[b][:, ti, :, :], ftk[:, tid, :, kc * 128:(kc + 1) * 128].rearrange("r p s -> p r s"), wr=[f"kt{b}"], sem=f"kt{b}")
            return kt[b], f"kt{b}"

        vdc = [0]

        def load_v(h0, nh, kc):
            b = vdc[0] % 2
            vdc[0] += 1
            D(vt[b][:, :, 0:nh, :], vk[:, kc * 128:(kc + 1) * 128, h0 * 128:(h0 + nh) * 128].rearrange("r s (h c) -> s r h c", c=128), wr=[f"vt{b}"], sem=f"vt{b}")
            return vt[b], f"vt{b}"

        def finalize(acc, accname, even, dst, dstname, ncol=512, c0=0):
            orow = slice(0, 64) if even else slice(64, 128)
            srow = slice(64, 128) if even else slice(0, 64)
            V(lambda e: e.reciprocal(out=rs[orow, 0:ncol], in_=acc[srow, c0:c0 + ncol]), rd=[accname], wr=["rs"])
            V(lambda e: e.tensor_tensor(out=dst, in0=acc[orow, c0:c0 + ncol], in1=rs[orow, 0:ncol], op=ALU.mult), rd=[accname, "rs"], wr=[dstname])

        sbanks = [(S0, "S0"), (S1, "S1")]
        sc = [0]

        def next_s():
            r = sbanks[sc[0] % len(sbanks)]
            sc[0] += 1
            return r

        ptc = [0]

        def next_pt():
            i = ptc[0] % 4
            ptc[0] += 1
            return PT[i], f"PT{i}"

        outs = []
        P(lambda e: e.memset(MIX[:], 0.0), wr=["MIX"])
        def qtile(g):
            D(QS[:, :, :], ftq[:, :, g * 512:(g + 1) * 512].rearrange("c p t -> p c t"), wr=["QS"], sem="QS")
            for h in range(4):
                V(lambda e, h=h: e.tensor_copy(out=QS[64:65, QB + h, :].rearrange("p (i t) -> p i t", t=128),
                                               in_=crefsc[64:65, 4 * g:4 * g + 4, h:h + 1].to_broadcast([1, 4, 128])),
                  rd=["QS", "crefsc"], wr=["QS"])
            for t_ in range(2):
                for mu in range(2):
                    V(lambda e, t_=t_, mu=mu: e.tensor_scalar(out=CQZ[:, t_, mu, :], in0=QS[:, QC + t_, :], scalar1=cstt[:, 6 + mu:7 + mu], scalar2=None, op0=ALU.mult),
                      rd=["QS", "cstt"], wr=["CQZ"])

            def dense_pass(acc_specs, ktiles, vh0, nvh, scale, bias_h=None):
                nkc = 4 * g + 4
                for kc in range(nkc):
                    KT_, ktn = load_k(ktiles, kc)
                    VT_, vtn = load_v(vh0, nvh, kc)
                    ip = kc - 4 * g
                    c0 = 128 * ip if ip > 0 else 0
                    ncol = 512 - c0
                    for r in range(4):
                        kb = 4 * kc + r
                        for (ai, kti, kr0, nrow, rhs_fn, vhi, bh) in acc_specs:
                            sbk, sname = next_s()
                            T(lambda e, sbk=sbk, KT_=KT_, kti=kti, kr0=kr0, nrow=nrow, r=r, rhs_fn=rhs_fn, c0=c0, ncol=ncol, ip=ip:
                              e.matmul(sbk[:, 0:ncol], KT_[kr0:kr0 + nrow, kti, r, :], rhs_fn(c0, ncol), start=True, stop=(ip < 0)),
                              rd=[ktn, "QS", "CQZ"], wr=[sname])
                            if ip >= 0:
                                T(lambda e, sbk=sbk, ip=ip, r=r, c0=c0, ncol=ncol: e.matmul(sbk[:, 0:ncol], ident[:, :], cm[:, 4 * ip + r, c0:c0 + ncol], start=False, stop=True),
                                  rd=["ident", "cm"], wr=[sname])
                            pt, ptn = next_pt()
                            if bh is not None:
                                A(lambda e, pt=pt, sbk=sbk, ncol=ncol, kb=kb, bh=bh: e.activation(out=pt[:, 0:ncol], in_=sbk[:, 0:ncol], func=AF.Exp, scale=scale, bias=nb[:, kb, bh:bh + 1]),
                                  rd=[sname, "nb"], wr=[ptn])
                            else:
                                A(lambda e, pt=pt, sbk=sbk, ncol=ncol: e.activation(out=pt[:, 0:ncol], in_=sbk[:, 0:ncol], func=AF.Exp, scale=scale), rd=[sname], wr=[ptn])
                            T(lambda e, ai=ai, VT_=VT_, r=r, vhi=vhi, pt=pt, c0=c0, ncol=ncol, kc=kc, nkc=nkc:
                              e.matmul(AC[ai][:, c0:512], VT_[:, r, vhi, :], pt[:, 0:ncol], start=(kc == 0 and r == 0), stop=(kc == nkc - 1 and r == 3)),
                              rd=[vtn, ptn], wr=[f"AC{ai}"])

            if "B" in groups:
                specs = []
                for h in range(4):
                    specs.append((h, h, 0, 65, (lambda c0, ncol, h=h: QS[0:65, QB + h, c0:c0 + ncol]), h, h))
                dense_pass(specs, [KB, KB + 1, KB + 2, KB + 3], VB, 4, SC_A)
                for h in range(4):
                    even = (h % 2 == 0)
                    rows = slice(0, 64) if even else slice(64, 128)
                    finalize(AC[h], f"AC{h}", even, MIX[rows, 2 + h // 2, :], "MIX")

            if "C" in groups:
                for t_ in range(2):
                    specs = []
                    for hh in range(2):
                        ph = 64 * hh
                        for mu in range(2):
                            specs.append((2 * hh + mu, 0, ph, 64, (lambda c0, ncol, t_=t_, mu=mu, ph=ph: CQZ[ph:ph + 64, t_, mu, c0:c0 + ncol]), hh, None))
                    dense_pass(specs, [KC + t_], VC + 2 * t_, 2, SC_C)
                    for hh in range(2):
                        even = (hh == 0)
                        rows = slice(0, 64) if even else slice(64, 128)
                        finalize(AC[2 * hh], f"AC{2 * hh}", even, o1[rows, :], "o1")
                        finalize(AC[2 * hh + 1], f"AC{2 * hh + 1}", even, o2[rows, :], "o2")
                    V(lambda e: e.scalar_tensor_tensor(out=o1[:], in0=o2[:], scalar=nlam[:, 0:1], in1=o1[:], op0=ALU.mult, op1=ALU.add), rd=["o1", "o2", "nlam"], wr=["o1"])
                    A(lambda e: e.activation(out=dsq[:], in_=o1[:], func=AF.Square), rd=["o1"], wr=["dsq"])
                    T(lambda e: e.matmul(X0[:, :], bones[:, :], dsq[:, :], start=True, stop=True), rd=["bones", "dsq"], wr=["X0"])
                    A(lambda e: e.activation(out=rt[:], in_=X0[:, :], func=AF.Sqrt, scale=1.0 / 64, bias=epst[:]), rd=["X0", "epst"], wr=["rt"])
                    V(lambda e: e.reciprocal(out=rs2[:], in_=rt[:]), rd=["rt"], wr=["rs2"])
                    V(lambda e: e.tensor_tensor(out=o1[:], in0=o1[:], in1=rs2[:], op=ALU.mult), rd=["o1", "rs2"], wr=["o1"])
                    V(lambda e, t_=t_: e.tensor_scalar(out=MIX[:, 4 + t_, :], in0=o1[:], scalar1=subeff[:, 0:1], scalar2=None, op0=ALU.mult), rd=["o1", "subeff"], wr=["MIX"])

            def qblock(i):
                m = 4 * g + i
                tcol = slice(i * 128, (i + 1) * 128)
                if "D" in groups:
                    b = ldc[0] % 2; ldc[0] += 1
                    bv = vdc[0] % 2; vdc[0] += 1
                    KT_, ktn, VT_, vtn = kt[b], f"kt{b}", vt[bv], f"vt{bv}"
                    for ti in range(2):
                        D(KT_[:, ti, :, :], ftk[:, KD + ti, :, m * 128:(m + 1) * 128].rearrange("r p s -> p r s"), wr=[ktn], sem=ktn)
                    D(VT_[:, :, 0:4, :], vk[:, m * 128:(m + 1) * 128, VD * 128:(VD + 4) * 128].rearrange("r s (h c) -> s r h c", c=128), wr=[vtn], sem=vtn)
                    ulist = [1, 2, 3, 4]
                    if m > 0:
                        ulist = [0, 1, 2, 3, 4]
                        for ti in range(2):
                            D(KT_[:, 2 + ti, 0, :], ftk[3, KD + ti, :, (m - 1) * 128:m * 128], wr=[ktn], sem=ktn)
                    if m > 0:
                        bv2 = vdc[0] % 2; vdc[0] += 1
                        VP_, vpn = vt[bv2], f"vt{bv2}"
                        D(VP_[:, 0, 0:4, :], vk[3, (m - 1) * 128:m * 128, VD * 128:(VD + 4) * 128].rearrange("s (h c) -> s h c", c=128), wr=[vpn], sem=vpn)
                    for hq in range(4):
                        ph = 64 * (hq % 2)
                        sbk, sname = next_s()
                        for ui, u in enumerate(ulist):
                            if u == 0:
                                lhs = KT_[ph:ph + 64, 2 + hq // 2, 0, :]
                            else:
                                lhs = KT_[ph:ph + 64, hq // 2, u - 1, :]
                            T(lambda e, sbk=sbk, lhs=lhs, ui=ui, hq=hq, ph=ph: e.matmul(sbk[:, ui * 128:(ui + 1) * 128] if ui < 4 else X0[:, 0:128], lhs, QS[ph:ph + 64, QD + hq // 2, tcol], start=True, stop=True),
                              rd=[ktn, "QS"], wr=[sname] if ui < 4 else ["X0"])
                        nu = len(ulist)
                        pt, ptn = next_pt()
                        pm = PM[hq % 2]; pmn = f"PM{hq % 2}"
                        n4 = min(nu, 4)
                        A(lambda e, pt=pt, sbk=sbk, n4=n4: e.activation(out=pt[:, 0:n4 * 128], in_=sbk[:, 0:n4 * 128], func=AF.Exp, scale=SC_A), rd=[sname], wr=[ptn])
                        P(lambda e, pt=pt, pm=pm, n4=n4, ulist=ulist: e.tensor_tensor(out=pm[:, 0:n4 * 128].rearrange("p (u t) -> p u t", t=128), in0=pt[:, 0:n4 * 128].rearrange("p (u t) -> p u t", t=128), in1=dm[:, ulist[0]:ulist[0] + n4, :], op=ALU.mult),
                          rd=[ptn, "dm"], wr=[pmn])
                        if nu == 5:
                            A(lambda e: e.activation(out=dl1[:], in_=X0[:, 0:128], func=AF.Exp, scale=SC_A), rd=["X0"], wr=["dl1"])
                            P(lambda e: e.tensor_tensor(out=dl2[:], in0=dl1[:], in1=dm[:, 4, :], op=ALU.mult), rd=["dl1", "dm"], wr=["dl2"])
                        vh = (hq // 2) * 2 + (hq % 2)
                        accD = AC[3]
                        for ui, u in enumerate(ulist):
                            if u == 0:
                                lhsv = VP_[:, 0, vh, :]
                                rd_v = [vpn]
                            else:
                                lhsv = VT_[:, u - 1, vh, :]
                                rd_v = [vtn]
                            if ui < 4:
                                rhs = pm[:, ui * 128:(ui + 1) * 128]
                                rdp = [pmn]
                            else:
                                rhs = dl2[:, :]
                                rdp = ["dl2"]
                            T(lambda e, hq=hq, lhsv=lhsv, rhs=rhs, ui=ui: e.matmul(accD[:, hq * 128:(hq + 1) * 128], lhsv, rhs, start=(ui == 0), stop=False), rd=rd_v + rdp, wr=["AC3"])
                        T(lambda e, hq=hq: e.matmul(accD[:, hq * 128:(hq + 1) * 128], skrow[0:1, hq, :], onesb[0:1, 0:128], start=False, stop=True), rd=["skrow", "onesb"], wr=["AC3"])
                    for hq in range(4):
                        even = (hq % 2 == 0)
                        rows = slice(0, 64) if even else slice(64, 128)
                        finalize(AC[3], "AC3", even, MIX[rows, 6 + hq // 2, tcol], "MIX", ncol=128, c0=hq * 128)

                if "A" in groups:
                    nkc = m + 1
                    L = nkc * 512
                    V(lambda e, m=m: e.tensor_tensor(out=diag[:], in0=ident[:, :].unsqueeze(1).to_broadcast([128, 8, 128]), in1=iwt[:, m, :].unsqueeze(2).to_broadcast([128, 8, 128]), op=ALU.mult),
                      rd=["ident", "iwt"], wr=["diag"])
                    for kc in range(nkc):
                        KT_, ktn = load_k([KI], kc)
                        for h in range(8):
                            ph = 64 * (h % 2)
                            sbk, sname = next_s()
                            T(lambda e, sbk=sbk, KT_=KT_, ph=ph, h=h: e.matmul(sbk[:, :], QS[ph:ph + 64, QI + h // 2, tcol], KT_[ph:ph + 64, 0, :, :].rearrange("p r s -> p (r s)"), start=True, stop=True),
                              rd=[ktn, "QS"], wr=[sname])
                            Rh = R[h % 4]; rn = f"R{h % 4}"
                            A(lambda e, Rh=Rh, sbk=sbk: e.activation(out=Rh[:], in_=sbk[:, :], func=AF.Relu), rd=[sname], wr=[rn])
                            T(lambda e, Rh=Rh, h=h: e.matmul(X0[:, :], diag[:, h, :], Rh[:, :], start=(h == 0), stop=(h == 7)), rd=[rn, "diag"], wr=["X0"])
                        if kc < nkc - 1:
                            A(lambda e, kc=kc: e.activation(out=score[:, kc * 512:(kc + 1) * 512], in_=X0[:, :], func=AF.Copy), rd=["X0"], wr=["score"])
                        else:
                            V(lambda e: e.tensor_tensor(out=lastm[:], in0=X0[:, :], in1=ixm[:, 1, :], op=ALU.add), rd=["X0", "ixm"], wr=["lastm"])
                            V(lambda e: e.tensor_reduce(out=mn1[:], in_=lastm[:], axis=AX.X, op=ALU.min), rd=["lastm"], wr=["mn1"])
                            V(lambda e, kc=kc: e.tensor_tensor(out=score[:, kc * 512:(kc + 1) * 512], in0=X0[:, :], in1=ixm[:, 0, :], op=ALU.add), rd=["X0", "ixm"], wr=["score"])
                    V(lambda e: e.tensor_reduce(out=hi[:], in_=score[:, 0:L], axis=AX.X, op=ALU.max), rd=["score"], wr=["hi"])
                    if nkc > 1:
                        V(lambda e: e.tensor_reduce(out=mn2[:], in_=score[:, 0:L - 512], axis=AX.X, op=ALU.min), rd=["score"], wr=["mn2"])
                        V(lambda e: e.tensor_tensor(out=lo[0][:], in0=mn1[:], in1=mn2[:], op=ALU.min), rd=["mn1", "mn2"], wr=["lo0"])
                    else:
                        V(lambda e: e.tensor_copy(out=lo[0][:], in_=mn1[:]), rd=["mn1"], wr=["lo0"])
                    V(lambda e: e.tensor_tensor(out=w0[:], in0=hi[:], in1=lo[0][:], op=ALU.subtract), rd=["hi", "lo0"], wr=["w0"])
                    V(lambda e: e.tensor_scalar(out=w0[:], in0=w0[:], scalar1=1.0001, scalar2=1e-6, op0=ALU.mult, op1=ALU.add), rd=["w0"], wr=["w0"])
                    V(lambda e: e.tensor_scalar(out=W[:], in0=pw[:], scalar1=w0[:, 0:1], scalar2=None, op0=ALU.mult), rd=["pw", "w0"], wr=["W"])
                    for k in range(NITER):
                        a, b2 = lo[k % 2], lo[(k + 1) % 2]
                        an, bn = f"lo{k % 2}", f"lo{(k + 1) % 2}"
                        V(lambda e, a=a, k=k: e.tensor_tensor(out=mid[:], in0=a[:], in1=W[:, k:k + 1], op=ALU.add), rd=[an, "W"], wr=["mid"])
                        V(lambda e: e.tensor_scalar(out=mk[:, 0:L], in0=score[:, 0:L], scalar1=mid[:, 0:1], scalar2=None, op0=ALU.is_ge, op1=ALU.add, accum_out=cnt[:]),
                          rd=["score", "mid"], wr=["mk", "cnt"])
                        V(lambda e: e.tensor_scalar(out=ge[:], in0=cnt[:], scalar1=255.5, scalar2=None, op0=ALU.is_ge), rd=["cnt"], wr=["ge"])
                        V(lambda e, k=k: e.tensor_tensor(out=gw[:], in0=ge[:], in1=W[:, k:k + 1], op=ALU.mult), rd=["ge", "W"], wr=["gw"])
                        V(lambda e, a=a, b2=b2: e.tensor_tensor(out=b2[:], in0=a[:], in1=gw[:], op=ALU.add), rd=[an, "gw"], wr=[bn])
                    thr = lo[NITER % 2]; thrn = f"lo{NITER % 2}"
                    V(lambda e, thr=thr: e.tensor_scalar(out=mk[:, 0:L], in0=score[:, 0:L], scalar1=thr[:, 0:1], scalar2=None, op0=ALU.is_ge), rd=["score", thrn], wr=["mk"])
                    nkb = 4 * nkc
                    for k0 in range(0, nkb, 8):
                        n8 = min(8, nkb - k0)
                        for k in range(n8):
                            T(lambda e, k0=k0, k=k: e.transpose(X1[:, k * 128:(k + 1) * 128], mk[:, (k0 + k) * 128:(k0 + k + 1) * 128], ident[:]), rd=["mk", "ident"], wr=["X1"])
                        if (k0 // 8) % 2 == 0:
                            A(lambda e, k0=k0, n8=n8: e.activation(out=mbt[:, k0:k0 + n8, :], in_=X1[:, 0:n8 * 128].rearrange("p (k t) -> p k t", t=128), func=AF.Copy), rd=["X1"], wr=["mbt"])
                        else:
                            V(lambda e, k0=k0, n8=n8: e.tensor_copy(out=mbt[:, k0:k0 + n8, :], in_=X1[:, 0:n8 * 128].rearrange("p (k t) -> p k t", t=128)), rd=["X1"], wr=["mbt"])
                    for kc in range(nkc):
                        KT_, ktn = load_k([KA, KA + 1], kc)
                        VT_, vtn = load_v(VA, 4, kc)
                        for h in range(4):
                            ph = 64 * (h % 2)
                            sbk, sname = next_s()
                            for r in range(4):
                                T(lambda e, sbk=sbk, KT_=KT_, ph=ph, h=h, r=r: e.matmul(sbk[:, r * 128:(r + 1) * 128], KT_[ph:ph + 64, h // 2, r, :], QS[ph:ph + 64, QA + h // 2, tcol], start=True, stop=True),
                                  rd=[ktn, "QS"], wr=[sname])
                            pt, ptn = next_pt()
                            pm = PM[h % 2]; pmn = f"PM{h % 2}"
                            A(lambda e, pt=pt, sbk=sbk: e.activation(out=pt[:], in_=sbk[:, :], func=AF.Exp, scale=SC_A), rd=[sname], wr=[ptn])
                            P(lambda e, pt=pt, pm=pm, kc=kc: e.tensor_tensor(out=pm[:], in0=pt[:], in1=mbt[:, 4 * kc:4 * kc + 4, :].rearrange("p k t -> p (k t)"), op=ALU.mult), rd=[ptn, "mbt"], wr=[pmn])
                            for r in range(4):
                                T(lambda e, h=h, VT_=VT_, r=r, pm=pm, kc=kc, nkc=nkc: e.matmul(AC[h][:, 0:128], VT_[:, r, h, :], pm[:, r * 128:(r + 1) * 128], start=(kc == 0 and r == 0), stop=(kc == nkc - 1 and r == 3)),
                                  rd=[vtn, pmn], wr=[f"AC{h}"])
                    for h in range(4):
                        even = (h % 2 == 0)
                        rows = slice(0, 64) if even else slice(64, 128)
                        finalize(AC[h], f"AC{h}", even, MIX[rows, h // 2, tcol], "MIX", ncol=128, c0=0)
            for i in range(4):
                qblock(i)
            outs.append(D(mixT[:, :, g * 512:(g + 1) * 512].rearrange("c p t -> p c t"), MIX[:, :, :], rd=["MIX"], sem="omix"))
        for g in gs:
            qtile(g)
        S.emit(final_waits=outs)
    return nc

import contextlib
import numpy as np
import concourse.bass as bass
import concourse.mybir as mybir

F32 = mybir.dt.float32
BF16 = mybir.dt.bfloat16
AF = mybir.ActivationFunctionType
ALU = mybir.AluOpType
NB = 16
D = 1024


def build_q2():
    nc = bass.Bass("TRN2", target_bir_lowering=False)
    I = lambda name, shape, dt=F32: nc.dram_tensor(name, shape, dt, kind="ExternalInput").ap()
    mixT = I("mixT", [8, 128, 2048], BF16)
    x = I("x", [2048, D])
    pin = I("pin", [2048, 256])
    w_out = I("w_out", [D, D]); w_up = I("w_up", [D, 4096]); w_dn = I("w_dn", [4096, D])
    w_pp = I("w_pp", [256, D]); w_pg = I("w_pg", [D, D])
    gpm = I("gpm", [1, D]); gpl = I("gpl", [1, D]); gmlp = I("gmlp", [128, 8])
    xo = nc.dram_tensor("xo", [2048, D], F32, kind="ExternalOutput").ap()
    es = contextlib.ExitStack()
    with es:
        def sb(name, shape, dt=F32):
            return es.enter_context(nc.sbuf_tensor(name, shape, dt))

        def ps(name, shape, dt=F32):
            return es.enter_context(nc.psum_tensor(name, shape, dt))

        S = Sched(nc)
        WB = sb("WB", [128, 65536], BF16)
        wst = [sb(f"wst{i}", [128, 1024]) for i in range(2)]
        gt = sb("gt", [128, 8]); gbc = sb("gbc", [128, D]); gbc2 = sb("gbc2", [128, D])
        identf = sb("identf", [128, 128]); ident = sb("ident", [128, 128], BF16)
        epst = sb("epst", [128, 1])
        xt = [sb(f"xt{i}", [128, D]) for i in range(2)]
        mt = [sb(f"mt{i}", [128, 8, 128], BF16) for i in range(2)]
        y = sb("y", [128, D]); tmp = sb("tmp", [128, D]); junk = sb("junk", [128, D], BF16)
        ss = sb("ss", [128, 1]); sq = sb("sq", [128, 1]); rstd = sb("rstd", [128, 1])
        hb = sb("hb", [128, D], BF16); hT = sb("hT", [128, D], BF16)
        UT = sb("UT", [128, 32, 128], BF16); rl = [sb(f"rl{i}", [128, 512], BF16) for i in range(2)]
        pt = sb("pt", [128, 256]); pb = sb("pb", [128, 256], BF16); pT = sb("pT", [128, 256], BF16)
        sg = sb("sg", [128, D])
        pp = [ps(f"pp{i}", [128, 512]) for i in range(2)]
        pu = [ps(f"pu{i}", [128, 512]) for i in range(2)]
        pTp = [ps(f"pTp{i}", [128, 1024], BF16) for i in range(2)]
        pq = [ps(f"pq{i}", [128, 512]) for i in range(2)]

        Dm = lambda o, i_, rd=(), wr=(), sem=None: S.op("sp", lambda e: e.dma_start(out=o, in_=i_), reads=rd, writes=wr, dsem=sem)
        P = lambda fn, rd=(), wr=(): S.op("pool", fn, reads=rd, writes=wr)
        V = lambda fn, rd=(), wr=(): S.op("dve", fn, reads=rd, writes=wr)
        A = lambda fn, rd=(), wr=(): S.op("act", fn, reads=rd, writes=wr)
        T = lambda fn, rd=(), wr=(): S.op("pe", fn, reads=rd, writes=wr)
        P(lambda e: e.memset(identf[:], 0.0), wr=["identf"])
        P(lambda e: e.affine_select(out=identf[:], in_=identf[:], pattern=[[-1, 128]], compare_op=ALU.not_equal, fill=1.0, base=0, channel_multiplier=1), rd=["identf"], wr=["identf"])
        V(lambda e: e.tensor_copy(out=ident[:], in_=identf[:]), rd=["identf"], wr=["ident"])
        P(lambda e: e.memset(epst[:], 1e-6), wr=["epst"])
        Dm(gt[:], gmlp[:, :], wr=["gt"], sem="c0")
        Dm(gbc[:], gpm[0:1, :].to_broadcast([128, D]), wr=["gbc"], sem="c1")
        Dm(gbc2[:], gpl[0:1, :].to_broadcast([128, D]), wr=["gbc2"], sem="c2")
        wc = [0]

        def load_w(src, nrow_chunks, ncols, woff, scale_g=False, tag="W"):
            for kc in range(nrow_chunks):
                for c0 in range(0, ncols, 1024):
                    i = wc[0] % 2; wc[0] += 1
                    st, sn = wst[i], f"wst{i}"
                    Dm(st[:, :], src[kc * 128:(kc + 1) * 128, c0:c0 + 1024], wr=[sn], sem=sn)
                    dst = WB[:, woff + kc * ncols + c0: woff + kc * ncols + c0 + 1024]
                    eng = "dve" if i == 0 else "pool"
                    if scale_g:
                        S.op(eng, lambda e, st=st, dst=dst, kc=kc: e.tensor_scalar(out=dst, in0=st[:, :], scalar1=gt[:, kc:kc + 1], scalar2=None, op0=ALU.mult), reads=[sn, "gt"], writes=[tag])
                    else:
                        S.op(eng, lambda e, st=st, dst=dst: e.tensor_copy(out=dst, in_=st[:, :]), reads=[sn], writes=[tag])

        def rms(src, srcname):
            A(lambda e: e.activation(out=junk[:], in_=src, func=AF.Square, accum_out=ss[:]), rd=[srcname], wr=["junk", "ss"])
            A(lambda e: e.activation(out=sq[:], in_=ss[:], func=AF.Sqrt, scale=1.0 / D, bias=epst[:]), rd=["ss", "epst"], wr=["sq"])
            V(lambda e: e.reciprocal(out=rstd[:], in_=sq[:]), rd=["sq"], wr=["rstd"])

        def norm_res(X, xn, g_tile, gname, m):
            rms(y[:], "y")
            V(lambda e: e.scalar_tensor_tensor(out=tmp[:], in0=y[:], scalar=rstd[:, 0:1], in1=g_tile[:], op0=ALU.mult, op1=ALU.mult), rd=["y", "rstd", gname], wr=["tmp"])
            V(lambda e: e.tensor_tensor(out=X[:], in0=X[:], in1=tmp[:], op=ALU.add), rd=["tmp", xn], wr=[xn])
            return Dm(xo[m * 128:(m + 1) * 128, :], X[:], rd=[xn], wr=[f"xo{m}"], sem=f"st{m % 2}")

        load_w(w_out, 8, D, 0, tag="W1")
        for m in range(NB):
            b = m % 2
            X, xn, M, mn = xt[b], f"xt{b}", mt[b], f"mt{b}"
            Dm(X[:], x[m * 128:(m + 1) * 128, :], wr=[xn], sem=xn)
            Dm(M[:], mixT[:, :, m * 128:(m + 1) * 128].rearrange("c p t -> p c t"), wr=[mn], sem=mn)
            for n in range(2):
                for c in range(8):
                    T(lambda e, M=M, n=n, c=c: e.matmul(pp[n][:, :], M[:, c, :], WB[:, c * D + n * 512: c * D + (n + 1) * 512], start=(c == 0), stop=(c == 7)),
                      rd=[mn, "W1"], wr=[f"pp{n}"])
                A(lambda e, n=n: e.activation(out=y[:, n * 512:(n + 1) * 512], in_=pp[n][:, :], func=AF.Copy), rd=[f"pp{n}"], wr=["y"])
            norm_res(X, xn, gbc, "gbc", m)
        UPO, DNO = 0, 32768
        load_w(w_up, 8, 4096, UPO, scale_g=True, tag="W1")
        load_w(w_dn, 32, D, DNO, tag="W2")
        for m in range(NB):
            b = m % 2
            X, xn = xt[b], f"xt{b}"
            Dm(X[:], xo[m * 128:(m + 1) * 128, :], rd=[f"xo{m}"], wr=[xn], sem=xn)
            rms(X[:], xn)
            A(lambda e, X=X: e.activation(out=hb[:], in_=X[:], func=AF.Copy, scale=rstd[:, 0:1]), rd=[xn, "rstd"], wr=["hb"])
            for kc in range(8):
                T(lambda e, kc=kc: e.transpose(pTp[0][:, kc * 128:(kc + 1) * 128], hb[:, kc * 128:(kc + 1) * 128], ident[:]), rd=["hb", "ident"], wr=["pTp0"])
            V(lambda e: e.tensor_copy(out=hT[:], in_=pTp[0][:]), rd=["pTp0"], wr=["hT"])
            for fg in range(8):
                pb_ = pu[fg % 2]; pn = f"pu{fg % 2}"
                for f4 in range(4):
                    fc = fg * 4 + f4
                    for kc in range(8):
                        T(lambda e, pb_=pb_, f4=f4, fc=fc, kc=kc: e.matmul(pb_[:, f4 * 128:(f4 + 1) * 128], WB[:, UPO + kc * 4096 + fc * 128: UPO + kc * 4096 + (fc + 1) * 128], hT[:, kc * 128:(kc + 1) * 128], start=(kc == 0), stop=(kc == 7)),
                          rd=["hT", "W1"], wr=[pn])
                r_ = rl[fg % 2]; rn = f"rl{fg % 2}"
                A(lambda e, r_=r_, pb_=pb_: e.activation(out=r_[:], in_=pb_[:, :], func=AF.Relu), rd=[pn], wr=[rn])
                P(lambda e, r_=r_, fg=fg: e.tensor_tensor(out=UT[:, fg * 4:(fg + 1) * 4, :].rearrange("p f t -> p (f t)"), in0=r_[:], in1=r_[:], op=ALU.mult), rd=[rn], wr=[f"UT{fg}"])
            for n in range(2):
                for fc in range(32):
                    T(lambda e, n=n, fc=fc: e.matmul(pp[n][:, :], UT[:, fc, :], WB[:, DNO + fc * D + n * 512: DNO + fc * D + (n + 1) * 512], start=(fc == 0), stop=(fc == 31)),
                      rd=[f"UT{fc // 4}", "W2"], wr=[f"pp{n}"])
                A(lambda e, n=n: e.activation(out=y[:, n * 512:(n + 1) * 512], in_=pp[n][:, :], func=AF.Copy), rd=[f"pp{n}"], wr=["y"])
            norm_res(X, xn, gbc2, "gbc2", m)
        GO, PO = 0, 8192
        load_w(w_pg, 8, D, GO, tag="W1")
        load_w(w_pp, 2, D, PO, tag="W1")
        outs = []
        for m in range(NB):
            b = m % 2
            X, xn = xt[b], f"xt{b}"
            Dm(X[:], xo[m * 128:(m + 1) * 128, :], rd=[f"xo{m}"], wr=[xn], sem=xn)
            Dm(pt[:], pin[m * 128:(m + 1) * 128, :], wr=["pt"], sem="pt")
            V(lambda e, X=X: e.tensor_copy(out=hb[:], in_=X[:]), rd=[xn], wr=["hb"])
            V(lambda e: e.tensor_copy(out=pb[:], in_=pt[:]), rd=["pt"], wr=["pb"])
            for kc in range(8):
                T(lambda e, kc=kc: e.transpose(pTp[0][:, kc * 128:(kc + 1) * 128], hb[:, kc * 128:(kc + 1) * 128], ident[:]), rd=["hb", "ident"], wr=["pTp0"])
            A(lambda e: e.activation(out=hT[:], in_=pTp[0][:], func=AF.Copy), rd=["pTp0"], wr=["hT"])
            for kc in range(2):
                T(lambda e, kc=kc: e.transpose(pTp[1][:, kc * 128:(kc + 1) * 128], pb[:, kc * 128:(kc + 1) * 128], ident[:]), rd=["pb", "ident"], wr=["pTp1"])
            V(lambda e: e.tensor_copy(out=pT[:], in_=pTp[1][:, 0:256]), rd=["pTp1"], wr=["pT"])
            for n in range(2):
                for kc in range(8):
                    T(lambda e, n=n, kc=kc: e.matmul(pp[n][:, :], hT[:, kc * 128:(kc + 1) * 128], WB[:, GO + kc * D + n * 512: GO + kc * D + (n + 1) * 512], start=(kc == 0), stop=(kc == 7)),
                      rd=["hT", "W1"], wr=[f"pp{n}"])
                for kc in range(2):
                    T(lambda e, n=n, kc=kc: e.matmul(pq[n][:, :], pT[:, kc * 128:(kc + 1) * 128], WB[:, PO + kc * D + n * 512: PO + kc * D + (n + 1) * 512], start=(kc == 0), stop=(kc == 1)),
                      rd=["pT", "W1"], wr=[f"pq{n}"])
                A(lambda e, n=n: e.activation(out=sg[:, n * 512:(n + 1) * 512], in_=pp[n][:, :], func=AF.Sigmoid), rd=[f"pp{n}"], wr=["sg"])
                V(lambda e, n=n: e.tensor_tensor(out=tmp[:, n * 512:(n + 1) * 512], in0=pq[n][:, :], in1=sg[:, n * 512:(n + 1) * 512], op=ALU.mult), rd=[f"pq{n}", "sg"], wr=["tmp"])
            V(lambda e, X=X: e.tensor_tensor(out=X[:], in0=X[:], in1=tmp[:], op=ALU.add), rd=["tmp", xn], wr=[xn])
            outs.append(Dm(xo[m * 128:(m + 1) * 128, :], X[:], rd=[xn], wr=[f"xo{m}"], sem=f"st{m % 2}"))
        S.emit(final_waits=outs)
    return nc

import math
import numpy as np
import ml_dtypes

BF = ml_dtypes.bfloat16
QSEL = [0, 1, 4, 5, 6, 7, 9, 10, 13, 14, 17, 18, 19, 20]
KSEL = [2, 3, 8, 11, 12, 15, 16, 21, 22, 23, 24]


def lam_init_of(L):
    return 0.8 - 0.6 * math.exp(-0.3 * L)


def core_masks(j):
    s = np.arange(128)[:, None]
    t = np.arange(128)[None, :]
    tri = (s <= t)
    cm = np.zeros((128, 16, 4, 128), np.float32)
    for ip in range(4):
        for r in range(4):
            for i in range(4):
                a, b = 4 * ip + r, 4 * i + j
                if a < b:
                    v = 0.0
                elif a == b:
                    v = np.where(tri, 0.0, -30000.0)
                else:
                    v = -30000.0
                cm[:, 4 * ip + r, i, :] = v
    cm = cm.reshape(128, 16, 512).astype(BF)
    ix = np.zeros((128, 2, 4, 128), np.float32)
    for r in range(4):
        if r < j:
            v = np.zeros((128, 128), np.float32)
        elif r == j:
            v = np.where(tri.T, 0.0, 1.0).astype(np.float32)
        else:
            v = np.ones((128, 128), np.float32)
        ix[:, 0, r, :] = v * -1e30
        ix[:, 1, r, :] = v * 1e30
    ix = ix.reshape(128, 2, 512)
    dm = np.zeros((128, 5, 128), np.float32)
    for u in range(5):
        if u == j:
            dm[:, u, :] = (s > t)
        elif u == j + 1:
            dm[:, u, :] = (s <= t)
    dm = dm.astype(BF)
    sel = np.zeros((64, 16), np.float32)
    for m in range(16):
        gb = 4 * m + j
        sel[:gb, m] = 1.0
        sel[gb, m] = 0.5
    return cm, ix, dm, sel


def q1_inputs(c, L, pres, inp):
    b, j = c // 4, c % 4
    ft = pres[c]["ft"]
    cm, ix, dm, sel = core_masks(j)
    ftk = np.stack([pres[4 * b + r]["ft"][KSEL] for r in range(4)], 0)
    vk = np.stack([pres[4 * b + r]["vv"] for r in range(4)], 0)
    lfa = np.zeros((128, 64, 4), np.float32)
    for r in range(4):
        sm = pres[4 * b + r]["sm"]
        lfa[:, r::4, :] = sm[:, 8:12].reshape(16, 128, 4).transpose(1, 0, 2)
    iwo = np.ascontiguousarray(pres[c]["sm"][:, 0:8].reshape(16, 128, 8).transpose(1, 0, 2))
    lamr = np.concatenate([inp["lambda_q1"][L], inp["lambda_q2"][L], inp["lambda_k1"][L], inp["lambda_k2"][L]])[None, :].astype(np.float32)
    cst = np.zeros((128, 9), np.float32)
    cst[:, 0:4] = inp["sinks"][L][None, :]
    li = lam_init_of(L)
    cst[:, 4] = li
    cst[:, 5] = np.tile(inp["diff_subln"][L], 2)
    p = np.arange(128) % 64
    cst[:, 6] = (p < 32)
    cst[:, 7] = (p >= 32)
    cst[:, 8] = 1.0 - li
    return {"ftq": np.ascontiguousarray(ft[QSEL]), "ftk": ftk, "vk": vk, "lfa": lfa, "iwo": iwo, "selb": sel,
            "cmask": cm, "idxm": ix, "dmask": dm, "lamr": lamr, "cst": cst}


def _core_tokens(c):
    b, j = c // 4, c % 4
    idx = np.concatenate([np.arange((4 * m + j) * 128, (4 * m + j + 1) * 128) for m in range(16)])
    return b, idx


def kernel(**inputs):
    from concourse.bass_utils import run_bass_kernel_spmd
    inp = {k: np.asarray(v) for k, v in inputs.items()}
    ncp, ncq1, ncq2 = build_p(), build_q1(), build_q2()
    cores = list(range(8))
    toks = [_core_tokens(c) for c in cores]
    xs = [np.ascontiguousarray(inp["x"][b][idx]).astype(np.float32) for (b, idx) in toks]
    poss = [np.ascontiguousarray(inp["positions"][b][idx].reshape(16, 128).T.astype(np.int32)) for (b, idx) in toks]
    invf = invf_table()
    for L in range(4):
        wperm = permute_w_in(inp["w_in"][L])
        g = np.ascontiguousarray(inp["norm_pre_mix"][L].reshape(8, 128).T)
        bfg = inp["b_forget"][L][None, :].astype(np.float32)
        pres = run_bass_kernel_spmd(ncp, [{"x": xs[c], "w": wperm, "g": g, "bfg": bfg, "pos": poss[c], "invf": invf} for c in cores], core_ids=cores).results
        q1 = run_bass_kernel_spmd(ncq1, [q1_inputs(c, L, pres, inp) for c in cores], core_ids=cores).results
        gm = np.ascontiguousarray(inp["norm_pre_mlp"][L].reshape(8, 128).T)
        maps = []
        for c in cores:
            b, idx = toks[c]
            maps.append({"mixT": q1[c]["mixT"], "x": xs[c], "pin": np.ascontiguousarray(inp["p"][L][b][idx]),
                         "w_out": inp["w_out"][L], "w_up": inp["w_mlp_up"][L], "w_dn": inp["w_mlp_down"][L],
                         "w_pp": inp["w_ple_proj"][L], "w_pg": inp["w_ple_gate"][L],
                         "gpm": inp["norm_post_mix"][L][None, :], "gpl": inp["norm_post_mlp"][L][None, :], "gmlp": gm})
        q2 = run_bass_kernel_spmd(ncq2, maps, core_ids=cores).results
        xs = [np.asarray(q2[c]["xo"], dtype=np.float32) for c in cores]
    out = np.zeros((2, 8192, 1024), np.float32)
    for c in cores:
        b, idx = toks[c]
        out[b][idx] = xs[c]
    return out
```
